# Optimizing a Trainium2 kernel written in Bass

```python
import jax
import jax.numpy as jnp
from jax import lax
import numpy as np

D_MODEL = 1024
BATCH = 2
SEQ = 16384
DEPTH = 2

CTX_LEN = 256
GRID_W = 64

S5_WIDTH = 512
S5_GROUP = 16
S5_GROUPS = S5_WIDTH // S5_GROUP
S5_STATE = 64
S5_STEP_MIN = 1e-3
S5_STEP_MAX = 1e-1
N_DIR = 2

CONV_WIDTH = 512
CONV_K = 31

N_BRANCHES = 2
OFF_CONV = S5_WIDTH
OFF_GATE = S5_WIDTH + 2 * CONV_WIDTH
IN_COLS = OFF_GATE + N_BRANCHES * D_MODEL

N_GROUPS = 4
EXPERTS_PER_GROUP = 8
N_EXPERTS = N_GROUPS * EXPERTS_PER_GROUP
TOP_K = 2
D_EXPERT = D_MODEL // 4

N_MOD = 6
NORM_EPS = 1e-6

kernel_name = 'hybrid_s5_conformer_hmoe_dit'


def rmsnorm(x, g):
    xf = x.astype(jnp.float32)
    y = xf * lax.rsqrt(jnp.mean(xf * xf, axis=-1, keepdims=True) + NORM_EPS)
    return (y * g.astype(jnp.float32)).astype(x.dtype)


def layernorm(x, g, b):
    xf = x.astype(jnp.float32)
    mu = jnp.mean(xf, axis=-1, keepdims=True)
    var = jnp.mean(jnp.square(xf - mu), axis=-1, keepdims=True)
    y = (xf - mu) * lax.rsqrt(var + NORM_EPS)
    return (y * g.astype(jnp.float32) + b.astype(jnp.float32)).astype(x.dtype)


def modulate(x, shift, scale):
    return x * (1 + scale) + shift


def glu(z):
    a, b = jnp.split(z, 2, axis=-1)
    return a * jax.nn.sigmoid(b)


def s5_discretize(lam_re, lam_im, log_step, b_re, b_im):
    lam = lax.complex(lam_re.astype(jnp.float32), lam_im.astype(jnp.float32))
    step = jnp.exp(log_step.astype(jnp.float32))[:, None]
    lam_bar = jnp.exp(lam * step)
    b = lax.complex(b_re.astype(jnp.float32), b_im.astype(jnp.float32))
    b_bar = ((lam_bar - 1) / lam)[..., None] * b
    return lam_bar, b_bar


def _ssm_combine(left, right):
    a_l, b_l = left
    a_r, b_r = right
    return a_r * a_l, a_r * b_l + b_r


def s5_scan(u, lam_bar, b_bar, h0, reverse):
    bu = lax.complex(jnp.einsum('blgh,gph->blgp', u, jnp.real(b_bar)),
                     jnp.einsum('blgh,gph->blgp', u, jnp.imag(b_bar)))
    if h0 is not None:
        edge = -1 if reverse else 0
        bu = bu.at[:, edge].add(lam_bar * h0)
    a = jnp.broadcast_to(lam_bar, (1, bu.shape[1]) + lam_bar.shape)
    _, h = lax.associative_scan(_ssm_combine, (a, bu), reverse=reverse, axis=1)
    return h


def s5_readout(h, c_re, c_im):
    return (jnp.einsum('blgp,ghp->blgh', jnp.real(h), c_re.astype(jnp.float32))
            - jnp.einsum('blgp,ghp->blgh', jnp.imag(h), c_im.astype(jnp.float32)))


def s5_mixer(u, uc, lam_re, lam_im, log_step, b_re, b_im, c_re, c_im, d_skip, ctx_out):
    n_b, n_l, _ = u.shape
    n_c = uc.shape[1]
    ug = u.astype(jnp.float32).reshape(n_b, n_l, S5_GROUPS, S5_GROUP)
    ucg = uc.astype(jnp.float32).reshape(n_b, n_c, S5_GROUPS, S5_GROUP)
    d = d_skip.astype(jnp.float32).reshape(S5_GROUPS, S5_GROUP)
    y = ug * d
    yc = ucg * d if ctx_out else None
    for direction in range(N_DIR):
        reverse = direction == 1
        lam_bar, b_bar = s5_discretize(lam_re[direction], lam_im[direction], log_step[direction],
                                       b_re[direction], b_im[direction])
        hc = s5_scan(ucg, lam_bar, b_bar, None, reverse)
        h0 = hc[:, 0] if reverse else hc[:, -1]
        h = s5_scan(ug, lam_bar, b_bar, h0, reverse)
        y = y + s5_readout(h, c_re[direction], c_im[direction])
        if ctx_out:
            yc = yc + s5_readout(hc, c_re[direction], c_im[direction])
    y = y.reshape(n_b, n_l, S5_WIDTH).astype(u.dtype)
    if ctx_out:
        yc = yc.reshape(n_b, n_c, S5_WIDTH).astype(uc.dtype)
    return y, yc


def s5_post(y, w_glu, w_out):
    return glu(jax.nn.gelu(y) @ w_glu) @ w_out


def conv_branch(zb, n_rows, w_dw, b_dw, ln_g, ln_b, w_out):
    v = glu(zb)
    n_b, n_l, ch = v.shape
    v = v.reshape(n_b * n_rows, n_l // n_rows, ch)
    v = lax.conv_general_dilated(v, w_dw.astype(v.dtype)[:, None, :], window_strides=(1,), padding='SAME',
                                 dimension_numbers=('NWC', 'WIO', 'NWC'), feature_group_count=ch) + b_dw
    v = jax.nn.silu(layernorm(v, ln_g, ln_b))
    return v.reshape(n_b, n_l, ch) @ w_out


def branch_merge(z, y_a, y_b, w_o):
    g_a, g_b = jnp.split(jax.nn.sigmoid(z[..., OFF_GATE:]), N_BRANCHES, axis=-1)
    return (g_a * y_a + g_b * y_b) @ w_o


def hier_moe(h, w_rg, b_rg, w_rx, b_rx, w_gate, w_up, w_down):
    hf = h.astype(jnp.float32)
    group_prob = jax.nn.softmax(hf @ w_rg.astype(jnp.float32) + b_rg.astype(jnp.float32), axis=-1)
    p_group, g_idx = lax.top_k(group_prob, 1)
    exp_logits = (hf @ w_rx.astype(jnp.float32) + b_rx.astype(jnp.float32)).reshape(-1, N_GROUPS, EXPERTS_PER_GROUP)
    in_group = jnp.take_along_axis(exp_logits, g_idx[:, :, None], axis=1)[:, 0]
    top_logits, top_idx = lax.top_k(in_group, TOP_K)
    weights = jax.nn.softmax(top_logits, axis=-1) * p_group
    expert_id = g_idx * EXPERTS_PER_GROUP + top_idx
    gate = jnp.sum(jax.nn.one_hot(expert_id, N_EXPERTS, dtype=jnp.float32) * weights[..., None], axis=1)
    gate = gate.astype(h.dtype)
    out = jnp.zeros_like(h)
    for e in range(N_EXPERTS):
        act = jax.nn.silu(h @ w_gate[e]) * (h @ w_up[e])
        out = out + gate[:, e:e + 1] * (act @ w_down[e])
    return out


def setup_inputs(seed: int = 0) -> dict:
    key = jax.random.key(seed)
    ks = iter(jax.random.split(key, 40))
    f32 = jnp.float32

    def nrm(shape, scale):
        return jax.random.normal(next(ks), shape, f32) * scale

    n_state = jnp.arange(S5_STATE, dtype=f32)
    return {
        'x': nrm((BATCH, SEQ, D_MODEL), 1.0),
        'c': nrm((BATCH, D_MODEL), 1.0),
        'ctx': nrm((BATCH, CTX_LEN, D_MODEL), 1.0),
        'c_ctx': nrm((D_MODEL,), 1.0),
        'w_ada': nrm((DEPTH, D_MODEL, N_MOD * D_MODEL), 0.5 * D_MODEL ** -0.5),
        'b_ada': nrm((DEPTH, N_MOD * D_MODEL), 0.01),
        'g_mix': 1.0 + nrm((DEPTH, D_MODEL), 0.02),
        'g_ffn': 1.0 + nrm((DEPTH, D_MODEL), 0.02),
        'w_in': nrm((DEPTH, D_MODEL, IN_COLS), D_MODEL ** -0.5),
        's5_lam_re': -0.5 + nrm((DEPTH, N_DIR, S5_GROUPS, S5_STATE), 0.01),
        's5_lam_im': jnp.pi * n_state + nrm((DEPTH, N_DIR, S5_GROUPS, S5_STATE), 0.01),
        's5_log_step': jax.random.uniform(next(ks), (DEPTH, N_DIR, S5_GROUPS), f32,
                                          minval=float(np.log(S5_STEP_MIN)), maxval=float(np.log(S5_STEP_MAX))),
        's5_b_re': nrm((DEPTH, N_DIR, S5_GROUPS, S5_STATE, S5_GROUP), (2 * S5_GROUP) ** -0.5),
        's5_b_im': nrm((DEPTH, N_DIR, S5_GROUPS, S5_STATE, S5_GROUP), (2 * S5_GROUP) ** -0.5),
        's5_c_re': nrm((DEPTH, N_DIR, S5_GROUPS, S5_GROUP, S5_STATE), 0.5),
        's5_c_im': nrm((DEPTH, N_DIR, S5_GROUPS, S5_GROUP, S5_STATE), 0.5),
        's5_d': nrm((DEPTH, S5_WIDTH), 0.5),
        'w_glu': nrm((DEPTH, S5_WIDTH, 2 * S5_WIDTH), S5_WIDTH ** -0.5),
        'w_a_out': nrm((DEPTH, S5_WIDTH, D_MODEL), S5_WIDTH ** -0.5),
        'conv_w': nrm((DEPTH, CONV_K, CONV_WIDTH), CONV_K ** -0.5),
        'conv_b': nrm((DEPTH, CONV_WIDTH), 0.01),
        'conv_ln_g': 1.0 + nrm((DEPTH, CONV_WIDTH), 0.02),
        'conv_ln_b': nrm((DEPTH, CONV_WIDTH), 0.01),
        'w_b_out': nrm((DEPTH, CONV_WIDTH, D_MODEL), CONV_WIDTH ** -0.5),
        'w_o': nrm((DEPTH, D_MODEL, D_MODEL), D_MODEL ** -0.5),
        'w_route_group': nrm((DEPTH, D_MODEL, N_GROUPS), D_MODEL ** -0.5),
        'b_route_group': nrm((DEPTH, N_GROUPS), 0.01),
        'w_route_expert': nrm((DEPTH, D_MODEL, N_EXPERTS), D_MODEL ** -0.5),
        'b_route_expert': nrm((DEPTH, N_EXPERTS), 0.01),
        'w_exp_gate': nrm((DEPTH, N_EXPERTS, D_MODEL, D_EXPERT), D_MODEL ** -0.5),
        'w_exp_up': nrm((DEPTH, N_EXPERTS, D_MODEL, D_EXPERT), D_MODEL ** -0.5),
        'w_exp_down': nrm((DEPTH, N_EXPERTS, D_EXPERT, D_MODEL), D_EXPERT ** -0.5),
        'g_final': 1.0 + nrm((D_MODEL,), 0.02),
    }


def reference(x, c, ctx, c_ctx, w_ada, b_ada, g_mix, g_ffn, w_in, s5_lam_re, s5_lam_im, s5_log_step,
              s5_b_re, s5_b_im, s5_c_re, s5_c_im, s5_d, w_glu, w_a_out, conv_w, conv_b, conv_ln_g, conv_ln_b,
              w_b_out, w_o, w_route_group, b_route_group, w_route_expert, b_route_expert,
              w_exp_gate, w_exp_up, w_exp_down, g_final):
    n_b, n_l, d = x.shape
    rows = n_l // GRID_W
    xc = ctx
    cond = jax.nn.silu(c)
    cond_ctx = jax.nn.silu(c_ctx)
    for l in range(DEPTH):
        last = l == DEPTH - 1
        sh1, sc1, gt1, sh2, sc2, gt2 = jnp.split((cond @ w_ada[l] + b_ada[l])[:, None, :], N_MOD, axis=-1)
        csh1, csc1, cgt1, csh2, csc2, cgt2 = jnp.split(cond_ctx @ w_ada[l] + b_ada[l], N_MOD)

        h = modulate(rmsnorm(x, g_mix[l]), sh1, sc1)
        hc = modulate(rmsnorm(xc, g_mix[l]), csh1, csc1)
        z = h @ w_in[l]
        zc = hc @ (w_in[l][:, :S5_WIDTH] if last else w_in[l])
        y_a, yc_a = s5_mixer(z[..., :S5_WIDTH], zc[..., :S5_WIDTH], s5_lam_re[l], s5_lam_im[l], s5_log_step[l],
                             s5_b_re[l], s5_b_im[l], s5_c_re[l], s5_c_im[l], s5_d[l], not last)
        y_a = s5_post(y_a, w_glu[l], w_a_out[l])
        y_b = conv_branch(z[..., OFF_CONV:OFF_GATE], rows, conv_w[l], conv_b[l], conv_ln_g[l], conv_ln_b[l], w_b_out[l])
        x = x + gt1 * branch_merge(z, y_a, y_b, w_o[l])
        if not last:
            yc_a = s5_post(yc_a, w_glu[l], w_a_out[l])
            yc_b = conv_branch(zc[..., OFF_CONV:OFF_GATE], 1, conv_w[l], conv_b[l], conv_ln_g[l], conv_ln_b[l], w_b_out[l])
            xc = xc + cgt1 * branch_merge(zc, yc_a, yc_b, w_o[l])

        h = modulate(rmsnorm(x, g_ffn[l]), sh2, sc2)
        if last:
            out = hier_moe(h.reshape(-1, d), w_route_group[l], b_route_group[l], w_route_expert[l],
                           b_route_expert[l], w_exp_gate[l], w_exp_up[l], w_exp_down[l])
            x = x + gt2 * out.reshape(x.shape)
        else:
            hc = modulate(rmsnorm(xc, g_ffn[l]), csh2, csc2)
            tokens = jnp.concatenate([h.reshape(-1, d), hc.reshape(-1, d)], axis=0)
            out = hier_moe(tokens, w_route_group[l], b_route_group[l], w_route_expert[l],
                           b_route_expert[l], w_exp_gate[l], w_exp_up[l], w_exp_down[l])
            n_lat = n_b * n_l
            x = x + gt2 * out[:n_lat].reshape(x.shape)
            xc = xc + cgt2 * out[n_lat:].reshape(xc.shape)
    return rmsnorm(x, g_final)
```

```python
import contextlib
import numpy as np
import concourse.bass as bass
import concourse.mybir as mybir
from concourse.bass_utils import run_bass_kernel_spmd

F32 = mybir.dt.float32
BF16 = mybir.dt.bfloat16
AF = mybir.ActivationFunctionType
ALU = mybir.AluOpType

D = 1024
KC = 8
SEQ = 16384
CTX = 256
NCORE = 8
EPS = 1e-6
IN_COLS = 3584


class MK:
    NDMA = 48

    def __init__(self):
        self.nc = bass.Bass("TRN2", target_bir_lowering=False)
        self.es = contextlib.ExitStack()
        nc = self.nc
        self.eng = {'pe': nc.tensor, 'act': nc.scalar, 'dve': nc.vector, 'pool': nc.gpsimd, 'sp': nc.sync}
        self.sem = {k: self.es.enter_context(nc.semaphore("s_" + k)) for k in ('pe', 'act', 'dve', 'pool')}
        self.cnt = {k: 0 for k in self.sem}
        self.dsem = [self.es.enter_context(nc.semaphore("d%d" % i)) for i in range(self.NDMA)]
        self.duse = [0] * self.NDMA
        self.dnext = 0
        self.waited = {k: {} for k in self.eng}
        self.lastw = {}
        self.readers = {}
        self.semobj = {}
        self.out_events = []
        self.nt = 0

    def sb(self, shape, dt, name=None):
        self.nt += 1
        return self.es.enter_context(self.nc.sbuf_tensor(name or "t%d" % self.nt, list(shape), dt))

    def ps(self, shape, dt, name=None):
        self.nt += 1
        return self.es.enter_context(self.nc.psum_tensor(name or "p%d" % self.nt, list(shape), dt))

    def dram(self, name, shape, dt, kind):
        return self.nc.dram_tensor(name, list(shape), dt, kind=kind).ap()

    def _wait(self, e, ev):
        sid, val = ev
        if self.waited[e].get(sid, 0) >= val:
            return
        self.eng[e].wait_ge(self.semobj[sid], val)
        self.waited[e][sid] = val

    def _deps(self, e, reads, writes):
        evs = []
        for r in reads:
            if r in self.lastw:
                evs.append(self.lastw[r])
        for w in writes:
            if w in self.lastw:
                evs.append(self.lastw[w])
            evs.extend(self.readers.get(w, []))
        for ev in evs:
            self._wait(e, ev)

    def _record(self, ev, reads, writes):
        for r in reads:
            self.readers.setdefault(r, []).append(ev)
        for w in writes:
            self.lastw[w] = ev
            self.readers[w] = []

    def op(self, e, fn, reads=(), writes=()):
        self._deps(e, reads, writes)
        ins = fn()
        self.cnt[e] += 1
        s = self.sem[e]
        ins.then_inc(s, 1)
        sid = 'c_' + e
        self.semobj[sid] = s
        ev = (sid, self.cnt[e])
        self._record(ev, reads, writes)
        return ev

    def dma(self, q, out, in_, reads=(), writes=(), is_output=False, **kw):
        i = self.dnext
        self.dnext = (self.dnext + 1) % self.NDMA
        sid = 'd%d' % i
        self.semobj[sid] = self.dsem[i]
        if self.duse[i] > 0:
            self._wait(q, (sid, 16 * self.duse[i]))
        self._deps(q, reads, writes)
        self.duse[i] += 1
        self.eng[q].dma_start(out=out, in_=in_, **kw).then_inc(self.dsem[i], 16)
        ev = (sid, 16 * self.duse[i])
        self._record(ev, reads, writes)
        if is_output:
            self.out_events.append(ev)
        return ev

    def finish(self):
        for ev in self.out_events:
            self._wait('sp', ev)
        self.es.close()
        return self.nc


def to_fm(a):
    n, f = a.shape
    return np.ascontiguousarray(a.T.reshape(f // 128, 128, n).transpose(1, 0, 2))


def from_fm(a):
    p, c, n = a.shape
    return np.ascontiguousarray(a.transpose(1, 0, 2).reshape(c * 128, n).T)


def rows_pm(w):
    k, n = w.shape
    return np.ascontiguousarray(w.reshape(k // 128, 128, n).transpose(1, 0, 2))


def col_pm(v):
    return np.ascontiguousarray(v.reshape(-1, 128).T)


_CACHE = {}


def run(nc, in_maps):
    res = run_bass_kernel_spmd(nc, in_maps, core_ids=list(range(NCORE)))
    return res.results


def build_ada():
    k = MK()
    nc = k.nc
    NS = 768
    condT = k.dram("condT", [128, KC, 3], F32, "ExternalInput")
    w = k.dram("w", [2, 128, KC, NS], F32, "ExternalInput")
    b = k.dram("b", [2, 1, NS], F32, "ExternalInput")
    out = k.dram("out", [2, 3, NS], F32, "ExternalOutput")
    ct = k.sb([128, KC, 3], F32)
    cs = k.sb([128, KC, 3], F32)
    ones = k.sb([1, 3], F32)
    k.dma('sp', ct[:], condT[:, :, :], writes=['ct'])
    k.op('act', lambda: nc.scalar.activation(out=cs[:], in_=ct[:], func=AF.Silu), reads=['ct'], writes=['cs'])
    k.op('dve', lambda: nc.vector.memset(ones[:], 1.0), writes=['ones'])
    for l in range(2):
        wt = k.sb([128, KC, NS], F32, name="wt%d" % l)
        bt = k.sb([1, NS], F32, name="bt%d" % l)
        ot = k.sb([3, NS], F32, name="ot%d" % l)
        k.dma('sp', wt[:], w[l], writes=['wt%d' % l])
        k.dma('sp', bt[:], b[l], writes=['bt%d' % l])
        for h in range(2):
            pt = k.ps([3, 384], F32, name="pa%d_%d" % (l, h))
            key = 'pa%d_%d' % (l, h)
            for c in range(KC):
                k.op('pe', lambda c=c: nc.tensor.matmul(pt[:], lhsT=cs[:, c, :], rhs=wt[:, c, h * 384:(h + 1) * 384],
                                                        start=(c == 0), stop=False),
                     reads=['cs', 'wt%d' % l], writes=[key])
            k.op('pe', lambda: nc.tensor.matmul(pt[:], lhsT=ones[:], rhs=bt[:, h * 384:(h + 1) * 384],
                                                start=False, stop=True),
                 reads=['ones', 'bt%d' % l], writes=[key])
            k.op('act', lambda: nc.scalar.copy(out=ot[:, h * 384:(h + 1) * 384], in_=pt[:]),
                 reads=[key], writes=['ot%d' % l])
        k.dma('sp', out[l], ot[:], reads=['ot%d' % l], is_output=True)
    return k.finish()


def run_ada(c, c_ctx, w_ada, b_ada):
    if 'ada' not in _CACHE:
        _CACHE['ada'] = build_ada()
    cond = np.stack([c[0], c[1], c_ctx], axis=0)
    condT = to_fm(cond)
    in_maps = []
    for k in range(NCORE):
        sl = slice(k * 768, (k + 1) * 768)
        w = np.stack([rows_pm(w_ada[l][:, sl]) for l in range(2)], axis=0)
        b = np.stack([b_ada[l][None, sl] for l in range(2)], axis=0)
        in_maps.append({"condT": condT, "w": np.ascontiguousarray(w), "b": np.ascontiguousarray(b)})
    res = run(_CACHE['ada'], in_maps)
    ada = np.concatenate([r["out"] for r in res], axis=2)
    return ada


def emit_norm_mod(k, x_t, xkey, h_t, hkey, nt, gs, sh, ones_bf, sq_t, rstd_t, tmp_t, ps_ss, eps_t, tag):
    nc = k.nc
    for c in range(KC):
        k.op('act', lambda c=c: nc.scalar.activation(out=sq_t[:, c, :nt], in_=x_t[:, c, :nt], func=AF.Square),
             reads=[xkey], writes=['sq' + tag])
    for c in range(KC):
        k.op('pe', lambda c=c: nc.tensor.matmul(ps_ss[:, :nt], lhsT=ones_bf[:], rhs=sq_t[:, c, :nt],
                                                start=(c == 0), stop=(c == KC - 1)),
             reads=['sq' + tag, 'ones_bf'], writes=['ps_ss' + tag])
    k.op('act', lambda: nc.scalar.activation(out=rstd_t[:, :nt], in_=ps_ss[:, :nt], func=AF.Sqrt,
                                             bias=eps_t[:, 0:1], scale=1.0 / D),
         reads=['ps_ss' + tag, 'eps'], writes=['rstd' + tag])
    k.op('dve', lambda: nc.vector.reciprocal(out=rstd_t[:, :nt], in_=rstd_t[:, :nt]),
         reads=['rstd' + tag], writes=['rstd' + tag])
    for c in range(KC):
        k.op('dve', lambda c=c: nc.vector.tensor_tensor(out=tmp_t[:, c, :nt], in0=x_t[:, c, :nt], in1=rstd_t[:, :nt],
                                                        op=ALU.mult),
             reads=[xkey, 'rstd' + tag], writes=['tmp%s_%d' % (tag, c)])
        k.op('act', lambda c=c: nc.scalar.activation(out=h_t[:, c, :nt], in_=tmp_t[:, c, :nt], func=AF.Identity,
                                                     bias=sh[:, c:c + 1], scale=gs[:, c:c + 1]),
             reads=['tmp%s_%d' % (tag, c), 'gs' + tag], writes=[hkey])


def emit_gs(k, g_t, sc_t, gs_t, tag):
    nc = k.nc
    k.op('dve', lambda: nc.vector.scalar_tensor_tensor(out=gs_t[:], in0=sc_t[:], scalar=1.0, in1=g_t[:],
                                                       op0=ALU.add, op1=ALU.mult),
         reads=['prm' + tag], writes=['gs' + tag])


def build_ka(ntok, ncols=IN_COLS):
    k = MK()
    nc = k.nc
    TT = min(512, ntok)
    nchunks = ntok // TT
    MC = ncols // 128
    xT = k.dram("xT", [128, KC, ntok], F32, "ExternalInput")
    prm = k.dram("prm", [128, 3, KC], F32, "ExternalInput")
    w = k.dram("w", [128, KC, ncols], F32, "ExternalInput")
    zT = k.dram("zT", [128, MC, ntok], F32, "ExternalOutput")

    wt = k.sb([128, KC, ncols], BF16)
    pt = k.sb([128, 3, KC], F32)
    gs = k.sb([128, KC], F32)
    ones_bf = k.sb([128, 128], BF16)
    eps_t = k.sb([128, 1], F32)
    xts = [k.sb([128, KC, TT], F32, name="x%d" % i) for i in range(2)]
    sq = k.sb([128, KC, TT], BF16)
    rstd = k.sb([128, TT], F32)
    tmp = k.sb([128, KC, TT], F32)
    h = k.sb([128, KC, TT], BF16)
    ps_ss = k.ps([128, 512], F32)
    pz = [k.ps([128, 512], F32, name="pz%d" % i) for i in range(4)]
    ZG = 4
    zo = [k.sb([128, ZG, TT], F32, name="zo%d" % i) for i in range(2)]

    k.dma('sp', pt[:], prm[:, :, :], writes=['prm'])
    for c in range(KC):
        k.dma('pool', wt[:, c, :], w[:, c, :], writes=['w%d' % c])
    k.op('dve', lambda: nc.vector.memset(ones_bf[:], 1.0), writes=['ones_bf'])
    k.op('dve', lambda: nc.vector.memset(eps_t[:], EPS), writes=['eps'])
    emit_gs(k, pt[:, 0, :], pt[:, 1, :], gs, '')
    nz = 0
    for ci in range(nchunks):
        xt = xts[ci % 2]
        xkey = 'x%d' % (ci % 2)
        k.dma('sp', xt[:], xT[:, :, ci * TT:(ci + 1) * TT], writes=[xkey])
        emit_norm_mod(k, xt, xkey, h, 'h', TT, gs, pt[:, 2, :], ones_bf, sq, rstd, tmp, ps_ss, eps_t, '')
        for m in range(MC):
            p = pz[m % 4]
            pkey = 'pz%d' % (m % 4)
            for c in range(KC):
                k.op('pe', lambda c=c, m=m, p=p: nc.tensor.matmul(p[:, :TT], lhsT=wt[:, c, m * 128:(m + 1) * 128],
                                                                   rhs=h[:, c, :], start=(c == 0), stop=(c == KC - 1)),
                     reads=['h', 'w%d' % c], writes=[pkey])
            g = m // ZG
            zb = zo[g % 2]
            zkey = 'zo%d' % (g % 2)
            eng = 'act' if m % 2 == 0 else 'dve'
            if eng == 'act':
                k.op('act', lambda p=p, zb=zb, m=m: nc.scalar.copy(out=zb[:, m % ZG, :], in_=p[:, :TT]),
                     reads=[pkey], writes=[zkey])
            else:
                k.op('dve', lambda p=p, zb=zb, m=m: nc.vector.tensor_copy(out=zb[:, m % ZG, :], in_=p[:, :TT]),
                     reads=[pkey], writes=[zkey])
            if m % ZG == ZG - 1:
                k.dma('sp', zT[:, g * ZG:(g + 1) * ZG, ci * TT:(ci + 1) * TT], zb[:], reads=[zkey], is_output=True)
    return k.finish()


def run_ka(x_list, prm_list, w_in_l, ntok):
    ncols = w_in_l.shape[1]
    key = ('ka', ntok, ncols)
    if key not in _CACHE:
        _CACHE[key] = build_ka(ntok, ncols)
    wl = rows_pm(w_in_l)
    in_maps = [{"xT": to_fm(x_list[k]), "prm": prm_list[k], "w": wl} for k in range(NCORE)]
    res = run(_CACHE[key], in_maps)
    return [r["zT"] for r in res]


def ada_cols(ada_l, row, idx):
    return col_pm(ada_l[row, idx * D:(idx + 1) * D])


NB1 = (SEQ + CTX) // 8
NU = 16
EXPS = [-j for j in range(8)] + list(range(9)) + [7 - j for j in range(8)] + \
       [8 * q for q in range(1, 8)] + [64 * q for q in range(1, 8)] + [512 * q for q in range(1, 8)] + \
       [4096 * q for q in range(1, 4)]
NE = len(EXPS)
IE1, IE2, IE3, IE8, IE64, IE512, IE4096 = 0, 8, 17, 25, 32, 39, 46
TWO_PI = 6.283185307179586
C1 = 6.28125
C2 = TWO_PI - C1
MAGIC = 12582912.0


def emit_consts(k):
    nc = k.nc
    I32 = mybir.dt.int32
    it = k.sb([128, 128], I32)
    itf = k.sb([128, 128], F32)
    ident = k.sb([128, 128], F32, name="ident")
    identb = k.sb([128, 128], BF16, name="identb")
    iswap = k.sb([128, 128], F32, name="iswap")
    mask = k.sb([128, 128], F32, name="mask")
    it2 = k.sb([128, 8, 16], I32)
    k.op('pool', lambda: nc.gpsimd.iota(it[:], pattern=[[1, 128]], base=0, channel_multiplier=-1), writes=['it'])
    k.op('dve', lambda: nc.vector.tensor_copy(out=itf[:], in_=it[:]), reads=['it'], writes=['itf'])
    k.op('dve', lambda: nc.vector.tensor_scalar(out=ident[:], in0=itf[:], scalar1=0.0, scalar2=None, op0=ALU.is_equal),
         reads=['itf'], writes=['ident'])
    k.op('dve', lambda: nc.vector.tensor_copy(out=identb[:], in_=ident[:]), reads=['ident'], writes=['identb'])
    k.op('dve', lambda: nc.vector.tensor_scalar(out=mask[:], in0=itf[:], scalar1=64.0, scalar2=None, op0=ALU.is_equal),
         reads=['itf'], writes=['mask'])
    k.op('dve', lambda: nc.vector.scalar_tensor_tensor(out=iswap[:], in0=itf[:], scalar=-64.0, in1=mask[:],
                                                       op0=ALU.is_equal, op1=ALU.add),
         reads=['itf', 'mask'], writes=['iswap'])
    k.op('pool', lambda: nc.gpsimd.iota(it2[:], pattern=[[16, 8], [0, 16]], base=15, channel_multiplier=-1),
         writes=['it2'])
    k.op('dve', lambda: nc.vector.tensor_copy(out=itf[:], in_=it2[:].rearrange("p a b -> p (a b)")),
         reads=['it2', 'ident', 'iswap', 'mask'], writes=['itf'])
    k.op('dve', lambda: nc.vector.tensor_scalar(out=mask[:], in0=itf[:], scalar1=0.0, scalar2=None, op0=ALU.is_ge),
         reads=['itf'], writes=['mask'])
    return ident, identb, iswap, mask


def build_kb():
    k = MK()
    nc = k.nc
    U = k.dram("U", [NU, 128, NB1], F32, "ExternalInput")
    lam = k.dram("lam", [128, 3, NU], F32, "ExternalInput")
    bc = k.dram("bc", [128, 4, NU, 16], F32, "ExternalInput")
    Y = k.dram("Y", [NU, 128, NB1], F32, "ExternalOutput")

    ident, identb, iswap, mask = emit_consts(k)
    lamt = k.sb([128, 3, NU], F32)
    bct = k.sb([128, 4, NU, 16], F32)
    k.dma('sp', lamt[:], lam[:, :, :], writes=['lamt'])
    k.dma('sp', bct[:], bc[:, :, :, :], writes=['bct'])

    def V(fn, reads, writes, e='dve'):
        return k.op(e, fn, reads=reads, writes=writes)

    step = k.sb([128, NU], F32)
    la = k.sb([128, NU], F32)
    th = k.sb([128, NU], F32)
    V(lambda: nc.scalar.activation(out=step[:], in_=lamt[:, 2, :], func=AF.Exp), ['lamt'], ['step'], 'act')
    V(lambda: nc.vector.tensor_tensor(out=la[:], in0=lamt[:, 0, :], in1=step[:], op=ALU.mult), ['lamt', 'step'], ['la'])
    V(lambda: nc.vector.tensor_tensor(out=th[:], in0=lamt[:, 1, :], in1=step[:], op=ALU.mult), ['lamt', 'step'], ['th'])
    LM = k.sb([128, NU, NE], F32)
    ANG = k.sb([128, NU, NE], F32)
    for i, e in enumerate(EXPS):
        V(lambda i=i, e=e: nc.vector.tensor_scalar(out=LM[:, :, i], in0=la[:], scalar1=float(e), scalar2=None,
                                                   op0=ALU.mult), ['la'], ['LM'])
        V(lambda i=i, e=e: nc.gpsimd.tensor_scalar(out=ANG[:, :, i], in0=th[:], scalar1=float(e), scalar2=None,
                                                   op0=ALU.mult), ['th'], ['ANG'], 'pool')
    MAG = k.sb([128, NU, NE], F32)
    V(lambda: nc.scalar.activation(out=MAG[:], in_=LM[:], func=AF.Exp), ['LM'], ['MAG'], 'act')
    T1 = k.sb([128, NU, NE], F32)
    T2 = k.sb([128, NU, NE], F32)
    RR = k.sb([128, NU, NE], F32)
    V(lambda: nc.vector.tensor_scalar(out=T1[:], in0=ANG[:], scalar1=1.0 / TWO_PI, scalar2=MAGIC,
                                      op0=ALU.mult, op1=ALU.add), ['ANG'], ['T1'])
    V(lambda: nc.vector.tensor_scalar(out=T2[:], in0=T1[:], scalar1=MAGIC, scalar2=None, op0=ALU.subtract),
      ['T1'], ['T2'])
    V(lambda: nc.vector.scalar_tensor_tensor(out=RR[:], in0=T2[:], scalar=-C1, in1=ANG[:], op0=ALU.mult, op1=ALU.add),
      ['T2', 'ANG'], ['RR'])
    V(lambda: nc.vector.scalar_tensor_tensor(out=RR[:], in0=T2[:], scalar=-C2, in1=RR[:], op0=ALU.mult, op1=ALU.add),
      ['T2', 'RR'], ['RR'])
    V(lambda: nc.vector.tensor_scalar(out=RR[:], in0=RR[:], scalar1=3.14159, scalar2=-3.14159, op0=ALU.min, op1=ALU.max),
      ['RR'], ['RR'])
    SN = k.sb([128, NU, NE], F32)
    S2 = k.sb([128, NU, NE], F32)
    CS = k.sb([128, NU, NE], F32)
    V(lambda: nc.scalar.activation(out=SN[:], in_=RR[:], func=AF.Sin), ['RR'], ['SN'], 'act')
    V(lambda: nc.scalar.activation(out=S2[:], in_=RR[:], func=AF.Sin, scale=0.5), ['RR'], ['S2'], 'act')
    V(lambda: nc.vector.tensor_tensor(out=CS[:], in0=S2[:], in1=S2[:], op=ALU.mult), ['S2'], ['CS'])
    V(lambda: nc.vector.tensor_scalar(out=CS[:], in0=CS[:], scalar1=-2.0, scalar2=1.0, op0=ALU.mult, op1=ALU.add),
      ['CS'], ['CS'])
    PR = k.sb([128, NU, NE], F32, name="PR")
    PI = k.sb([128, NU, NE], F32, name="PI")
    V(lambda: nc.vector.tensor_tensor(out=PR[:], in0=MAG[:], in1=CS[:], op=ALU.mult), ['MAG', 'CS'], ['PR'])
    V(lambda: nc.vector.tensor_tensor(out=PI[:], in0=MAG[:], in1=SN[:], op=ALU.mult), ['MAG', 'SN'], ['PI'])
    TA = k.sb([128, NU, NE], F32, name="TA")
    TB = k.sb([128, NU, NE], F32, name="TB")
    TD = k.sb([128, NU, NE], F32, name="TD")
    TE = k.sb([128, NU, NE], F32, name="TE")
    H0, H1 = slice(0, 64), slice(64, 128)
    V(lambda: nc.vector.tensor_copy(out=TA[H0], in_=PR[H0]), ['PR'], ['TA'])
    V(lambda: nc.vector.tensor_scalar(out=TA[H1], in0=PI[H1], scalar1=-1.0, scalar2=None, op0=ALU.mult), ['PI'], ['TA'])
    V(lambda: nc.vector.tensor_copy(out=TB[H0], in_=PI[H0]), ['PI'], ['TB'])
    V(lambda: nc.vector.tensor_copy(out=TB[H1], in_=PR[H1]), ['PR'], ['TB'])
    V(lambda: nc.vector.tensor_copy(out=TD[H0], in_=PI[H0]), ['PI'], ['TD'])
    V(lambda: nc.vector.tensor_scalar(out=TD[H1], in0=PR[H1], scalar1=-1.0, scalar2=None, op0=ALU.mult), ['PR'], ['TD'])
    V(lambda: nc.vector.tensor_copy(out=TE[H0], in_=PI[H0]), ['PI'], ['TE'])
    V(lambda: nc.vector.tensor_scalar(out=TE[H1], in0=PI[H1], scalar1=-1.0, scalar2=None, op0=ALU.mult), ['PI'], ['TE'])
    TC = TB
    TCt = k.sb([128, NU, NE], F32, name="TC")
    V(lambda: nc.vector.tensor_copy(out=TCt[H0], in_=PR[H0]), ['PR'], ['TC'])
    V(lambda: nc.vector.tensor_copy(out=TCt[H1], in_=PI[H1]), ['PI'], ['TC'])

    i1 = IE2 + 1
    nr = k.sb([128, NU], F32)
    den = k.sb([128, NU], F32)
    t3 = k.sb([128, NU], F32)
    qre = k.sb([128, NU], F32)
    qim = k.sb([128, NU], F32)
    lr, li = lamt[:, 0, :], lamt[:, 1, :]
    V(lambda: nc.vector.tensor_scalar(out=nr[:], in0=PR[:, :, i1], scalar1=-1.0, scalar2=None, op0=ALU.add), ['PR'], ['nr'])
    V(lambda: nc.vector.tensor_tensor(out=den[:], in0=lr, in1=lr, op=ALU.mult), ['lamt'], ['den'])
    V(lambda: nc.vector.tensor_tensor(out=t3[:], in0=li, in1=li, op=ALU.mult), ['lamt'], ['t3'])
    V(lambda: nc.vector.tensor_tensor(out=den[:], in0=den[:], in1=t3[:], op=ALU.add), ['den', 't3'], ['den'])
    V(lambda: nc.vector.reciprocal(out=den[:], in_=den[:]), ['den'], ['den'])
    V(lambda: nc.vector.tensor_tensor(out=qre[:], in0=nr[:], in1=lr, op=ALU.mult), ['nr', 'lamt'], ['qre'])
    V(lambda: nc.vector.tensor_tensor(out=t3[:], in0=PI[:, :, i1], in1=li, op=ALU.mult), ['PI', 'lamt', 'den'], ['t3'])
    V(lambda: nc.vector.tensor_tensor(out=qre[:], in0=qre[:], in1=t3[:], op=ALU.add), ['qre', 't3'], ['qre'])
    V(lambda: nc.vector.tensor_tensor(out=qre[:], in0=qre[:], in1=den[:], op=ALU.mult), ['qre', 'den'], ['qre'])
    V(lambda: nc.vector.tensor_tensor(out=qim[:], in0=PI[:, :, i1], in1=lr, op=ALU.mult), ['PI', 'lamt'], ['qim'])
    V(lambda: nc.vector.tensor_tensor(out=t3[:], in0=nr[:], in1=li, op=ALU.mult), ['nr', 'lamt', 'qre'], ['t3'])
    V(lambda: nc.vector.tensor_tensor(out=qim[:], in0=qim[:], in1=t3[:], op=ALU.subtract), ['qim', 't3'], ['qim'])
    V(lambda: nc.vector.tensor_tensor(out=qim[:], in0=qim[:], in1=den[:], op=ALU.mult), ['qim', 'den'], ['qim'])
    bbr = k.sb([128, NU, 16], F32)
    bbi = k.sb([128, NU, 16], F32)
    t4 = k.sb([128, NU, 16], F32)
    qre_b = qre[:].unsqueeze(2).to_broadcast([128, NU, 16])
    qim_b = qim[:].unsqueeze(2).to_broadcast([128, NU, 16])
    V(lambda: nc.vector.tensor_tensor(out=bbr[:], in0=bct[:, 0], in1=qre_b, op=ALU.mult), ['bct', 'qre'], ['bbr'])
    V(lambda: nc.vector.tensor_tensor(out=t4[:], in0=bct[:, 1], in1=qim_b, op=ALU.mult), ['bct', 'qim'], ['t4'])
    V(lambda: nc.vector.tensor_tensor(out=bbr[:], in0=bbr[:], in1=t4[:], op=ALU.subtract), ['bbr', 't4'], ['bbr'])
    V(lambda: nc.vector.tensor_tensor(out=bbi[:], in0=bct[:, 1], in1=qre_b, op=ALU.mult), ['bct', 'qre'], ['bbi'])
    V(lambda: nc.vector.tensor_tensor(out=t4[:], in0=bct[:, 0], in1=qim_b, op=ALU.mult), ['bct', 'qim', 'bbr'], ['t4'])
    V(lambda: nc.vector.tensor_tensor(out=bbi[:], in0=bbi[:], in1=t4[:], op=ALU.add), ['bbi', 't4'], ['bbi'])

    CO = k.sb([128, NU, 9, 16], F32, name="CO")
    COb = k.sb([128, NU, 9, 16], BF16, name="COb")
    BX = k.sb([128, NU, 8, 16], F32, name="BX")
    RMT = k.sb([128, NU, 8, 16], F32, name="RMT")
    t5 = k.sb([128, NU, 9, 16], F32)

    def bc_h(t, n):
        return t.unsqueeze(2).to_broadcast([128, NU, n, 16])

    def bc_e(t, i0, n):
        return t[:, :, i0:i0 + n].unsqueeze(3).to_broadcast([128, NU, n, 16])

    V(lambda: nc.vector.tensor_tensor(out=CO[:], in0=bc_h(bct[:, 2], 9), in1=bc_e(TA, IE2, 9), op=ALU.mult),
      ['bct', 'TA'], ['CO'])
    V(lambda: nc.vector.tensor_tensor(out=t5[:], in0=bc_h(bct[:, 3], 9), in1=bc_e(TB, IE2, 9), op=ALU.mult),
      ['bct', 'TB'], ['t5'])
    V(lambda: nc.vector.tensor_tensor(out=CO[:], in0=CO[:], in1=t5[:], op=ALU.subtract), ['CO', 't5'], ['CO'])
    V(lambda: nc.vector.tensor_copy(out=COb[:], in_=CO[:]), ['CO'], ['COb'])
    for (dst, dkey, i0) in ((BX, 'BX', IE1), (RMT, 'RMT', IE3)):
        V(lambda dst=dst, i0=i0: nc.vector.tensor_tensor(out=dst[:], in0=bc_h(bbr[:], 8), in1=bc_e(TCt, i0, 8),
                                                         op=ALU.mult), ['bbr', 'TC'], [dkey])
        V(lambda i0=i0: nc.vector.tensor_tensor(out=t5[:, :, 0:8, :], in0=bc_h(bbi[:], 8), in1=bc_e(TD, i0, 8),
                                                op=ALU.mult), ['bbi', 'TD', 'CO', dkey], ['t5'])
        V(lambda dst=dst: nc.vector.tensor_tensor(out=dst[:], in0=dst[:], in1=t5[:, :, 0:8, :], op=ALU.subtract),
          [dkey, 't5'], [dkey])

    NSLOT = 2
    NROT = 24
    rot = [k.sb([128, NROT, 128], BF16, name="rot%d" % s) for s in range(NSLOT)]
    rtmp = [k.sb([128, 128], F32, name="rtmp%d" % s) for s in range(NSLOT)]
    Mi = [k.sb([128, 128], BF16, name="Mi%d" % s) for s in range(NSLOT)]
    Rm = [k.sb([128, 128], BF16, name="Rm%d" % s) for s in range(NSLOT)]
    Ub = [k.sb([128, NB1], BF16, name="Ub%d" % s) for s in range(NSLOT)]
    S1b = [k.sb([128, NB1], BF16, name="S1b%d" % s) for s in range(NSLOT)]
    H1b = [k.sb([128, NB1], BF16, name="H1b%d" % s) for s in range(NSLOT)]
    S2b = [k.sb([128, 264], BF16, name="S2b%d" % s) for s in range(NSLOT)]
    H2b = [k.sb([128, 264], BF16, name="H2b%d" % s) for s in range(NSLOT)]
    S3b = [k.sb([128, 40], BF16, name="S3b%d" % s) for s in range(NSLOT)]
    H3b = [k.sb([128, 40], BF16, name="H3b%d" % s) for s in range(NSLOT)]
    S4b = [k.sb([128, 8], BF16, name="S4b%d" % s) for s in range(NSLOT)]
    H4b = [k.sb([128, 8], BF16, name="H4b%d" % s) for s in range(NSLOT)]
    Yo = [k.sb([128, NB1], F32, name="Yo%d" % s) for s in range(NSLOT)]
    pp = [k.ps([128, 512], F32, name="pp%d" % s) for s in range(NSLOT)]
    pl = [[k.ps([128, 512], F32, name="pl%d_%d" % (s, i)) for i in range(2)] for s in range(NSLOT)]
    py = [k.ps([128, 512], F32, name="py%d" % s) for s in range(NSLOT)]
    for s in range(NSLOT):
        k.op('pool', lambda s=s: nc.gpsimd.memset(S2b[s][:], 0.0), writes=['S2b%d' % s])
        k.op('pool', lambda s=s: nc.gpsimd.memset(S3b[s][:], 0.0), writes=['S3b%d' % s])
        k.op('pool', lambda s=s: nc.gpsimd.memset(H4b[s][:], 0.0), writes=['H4b%d' % s])

    chunks = [(c0, min(512, NB1 - c0)) for c0 in range(0, NB1, 512)]

    def unit_gen(u, s):
        sk = str(s)
        k.dma('pool', Ub[s][:], U[u], writes=['Ub' + sk])
        for r in range(NROT):
            ei = IE8 + r
            k.op('dve', lambda ei=ei: nc.vector.tensor_scalar(out=rtmp[s][:], in0=iswap[:], scalar1=TE[:, u, ei:ei + 1],
                                                             scalar2=None, op0=ALU.mult),
                 reads=['iswap', 'TE'], writes=['rtmp' + sk])
            k.op('dve', lambda ei=ei, r=r: nc.vector.scalar_tensor_tensor(out=rot[s][:, r, :], in0=ident[:],
                                                                          scalar=PR[:, u, ei:ei + 1], in1=rtmp[s][:],
                                                                          op0=ALU.mult, op1=ALU.add),
                 reads=['ident', 'PR', 'rtmp' + sk], writes=['rot' + sk])
        yield
        k.op('pe', lambda: nc.tensor.matmul(pp[s][:, 0:128], lhsT=BX[:, u].rearrange("p a b -> p (a b)"),
                                            rhs=CO[:, u, 0:8, :].rearrange("p a b -> p (a b)"), start=True, stop=True),
             reads=['BX', 'CO'], writes=['pp' + sk])
        k.op('dve', lambda: nc.vector.tensor_tensor(out=Mi[s][:], in0=pp[s][:, 0:128], in1=mask[:], op=ALU.mult),
             reads=['pp' + sk, 'mask'], writes=['Mi' + sk])
        k.op('pe', lambda: nc.tensor.matmul(pp[s][:, 128:256], lhsT=RMT[:, u].rearrange("p a b -> p (a b)"),
                                            rhs=ident[:], start=True, stop=True),
             reads=['RMT', 'ident'], writes=['ppb' + sk])
        k.op('act', lambda: nc.scalar.copy(out=Rm[s][:], in_=pp[s][:, 128:256]), reads=['ppb' + sk], writes=['Rm' + sk])
        yield

        def rmat(lvl, q):
            if q == 0:
                return identb[:]
            return rot[s][:, lvl * 7 + q - 1, :]

        for (c0, cn) in chunks:
            k.op('pe', lambda c0=c0, cn=cn: nc.tensor.matmul(py[s][:, :cn], lhsT=Rm[s][:], rhs=Ub[s][:, c0:c0 + cn],
                                                             start=True, stop=True),
                 reads=['Rm' + sk, 'Ub' + sk], writes=['py' + sk])
            k.op('act', lambda c0=c0, cn=cn: nc.scalar.copy(out=S1b[s][:, c0:c0 + cn], in_=py[s][:, :cn]),
                 reads=['py' + sk], writes=['S1b' + sk])
            yield
        npl = [0]

        def nextpl():
            i = npl[0] % 2
            npl[0] += 1
            return pl[s][i], 'pl%s_%d' % (sk, i)

        def summarize(Sb, skey, n, lvl, Sout, sokey):
            p, pk = nextpl()
            v = Sb[:, 0:n * 8].rearrange("p (m j) -> p m j", j=8)
            for j in range(8):
                k.op('pe', lambda j=j: nc.tensor.matmul(p[:, :n], lhsT=rmat(lvl, 7 - j), rhs=v[:, :, j],
                                                        start=(j == 0), stop=(j == 7)),
                     reads=[skey, 'rot' + sk, 'identb'], writes=[pk])
            k.op('act', lambda: nc.scalar.copy(out=Sout[:, 0:n], in_=p[:, :n]), reads=[pk], writes=[sokey])

        def expand(Sb, skey, n, lvl, Hp, hpkey, Hout, hokey):
            v = Sb[:, 0:n * 8].rearrange("p (m j) -> p m j", j=8)
            vo = Hout[:, 0:n * 8].rearrange("p (m j) -> p m j", j=8)
            for J in range(8):
                p, pk = nextpl()
                k.op('pe', lambda J=J: nc.tensor.matmul(p[:, :n], lhsT=rmat(lvl, J), rhs=Hp[:, 0:n],
                                                        start=True, stop=(J == 0)),
                     reads=[hpkey, 'rot' + sk, 'identb'], writes=[pk])
                for Jp in range(J):
                    k.op('pe', lambda J=J, Jp=Jp: nc.tensor.matmul(p[:, :n], lhsT=rmat(lvl, J - 1 - Jp), rhs=v[:, :, Jp],
                                                                   start=False, stop=(Jp == J - 1)),
                         reads=[skey, 'rot' + sk, 'identb'], writes=[pk])
                eng = 'act' if J % 2 == 0 else 'dve'
                if eng == 'act':
                    k.op('act', lambda J=J, p=p: nc.scalar.copy(out=vo[:, :, J], in_=p[:, :n]), reads=[pk], writes=[hokey])
                else:
                    k.op('dve', lambda J=J, p=p: nc.vector.tensor_copy(out=vo[:, :, J], in_=p[:, :n]), reads=[pk],
                         writes=[hokey])

        summarize(S1b[s], 'S1b' + sk, 260, 0, S2b[s], 'S2b' + sk)
        yield
        summarize(S2b[s], 'S2b' + sk, 33, 1, S3b[s], 'S3b' + sk)
        yield
        summarize(S3b[s], 'S3b' + sk, 5, 2, S4b[s], 'S4b' + sk)
        yield

        def r4096(q):
            if q == 0:
                return identb[:]
            return rot[s][:, 21 + q - 1, :]
        for J in range(1, 5):
            p, pk = nextpl()
            for Jp in range(J):
                k.op('pe', lambda J=J, Jp=Jp: nc.tensor.matmul(p[:, 0:1], lhsT=r4096(J - 1 - Jp), rhs=S4b[s][:, Jp:Jp + 1],
                                                               start=(Jp == 0), stop=(Jp == J - 1)),
                     reads=['S4b' + sk, 'rot' + sk, 'identb'], writes=[pk])
            k.op('act', lambda J=J, p=p: nc.scalar.copy(out=H4b[s][:, J:J + 1], in_=p[:, 0:1]), reads=[pk],
                 writes=['H4b' + sk])
        yield
        expand(S3b[s], 'S3b' + sk, 5, 2, H4b[s], 'H4b' + sk, H3b[s], 'H3b' + sk)
        yield
        expand(S2b[s], 'S2b' + sk, 33, 1, H3b[s], 'H3b' + sk, H2b[s], 'H2b' + sk)
        yield
        expand(S1b[s], 'S1b' + sk, 260, 0, H2b[s], 'H2b' + sk, H1b[s], 'H1b' + sk)
        yield
        for (c0, cn) in chunks:
            k.op('pe', lambda c0=c0, cn=cn: nc.tensor.matmul(py[s][:, :cn], lhsT=Mi[s][:], rhs=Ub[s][:, c0:c0 + cn],
                                                             start=True, stop=False),
                 reads=['Mi' + sk, 'Ub' + sk], writes=['py' + sk])
            k.op('pe', lambda c0=c0, cn=cn: nc.tensor.matmul(py[s][:, :cn],
                                                             lhsT=COb[:, u, 1:9, :].rearrange("p a b -> p (a b)"),
                                                             rhs=H1b[s][:, c0:c0 + cn], start=False, stop=True),
                 reads=['COb', 'H1b' + sk], writes=['py' + sk])
            k.op('dve', lambda c0=c0, cn=cn: nc.vector.tensor_copy(out=Yo[s][:, c0:c0 + cn], in_=py[s][:, :cn]),
                 reads=['py' + sk], writes=['Yo' + sk])
            yield
        k.dma('sp', Y[u], Yo[s][:], reads=['Yo' + sk], is_output=True)
        yield

    pending = list(range(NU))
    active = []
    while pending or active:
        while pending and len(active) < NSLOT:
            u = pending.pop(0)
            used = {a[1] for a in active}
            s = [x for x in range(NSLOT) if x not in used][0]
            active.append((unit_gen(u, s), s))
        for a in list(active):
            try:
                next(a[0])
            except StopIteration:
                active.remove(a)
    return k.finish()


def s5_units_in(z_lat, z_ctx, b, d, g):
    seq = np.concatenate([z_ctx[b][:, g * 16:(g + 1) * 16], z_lat[b][:, g * 16:(g + 1) * 16]], axis=0)
    if d == 1:
        seq = np.concatenate([z_ctx[b][::-1, g * 16:(g + 1) * 16], z_lat[b][::-1, g * 16:(g + 1) * 16]], axis=0)
    return seq.reshape(NB1, 8, 16).transpose(1, 2, 0).reshape(128, NB1)


def run_kb(l, z_lat, z_ctx, inp):
    if 'kb' not in _CACHE:
        _CACHE['kb'] = build_kb()
    in_maps = []
    for k in range(NCORE):
        b, d, g0 = k // 4, (k % 4) // 2, (k % 2) * 16
        Uc = np.stack([s5_units_in(z_lat, z_ctx, b, d, g0 + u) for u in range(NU)], axis=0)

        def pcol(a):
            return np.concatenate([a.T, a.T], axis=0)
        gs = slice(g0, g0 + NU)
        lam = np.stack([pcol(inp['s5_lam_re'][l, d, gs]), pcol(inp['s5_lam_im'][l, d, gs]),
                        np.broadcast_to(inp['s5_log_step'][l, d, gs][None, :], (128, NU))], axis=1)
        bre = inp['s5_b_re'][l, d, gs].transpose(1, 0, 2)
        bim = inp['s5_b_im'][l, d, gs].transpose(1, 0, 2)
        cre = inp['s5_c_re'][l, d, gs].transpose(2, 0, 1)
        cim = inp['s5_c_im'][l, d, gs].transpose(2, 0, 1)
        bc = np.stack([np.concatenate([t, t], axis=0) for t in (bre, bim, cre, cim)], axis=1)
        in_maps.append({"U": np.ascontiguousarray(Uc, dtype=np.float32), "lam": np.ascontiguousarray(lam, dtype=np.float32),
                        "bc": np.ascontiguousarray(bc, dtype=np.float32)})
    res = run(_CACHE['kb'], in_maps)
    y = np.empty((2, 2, SEQ, 512), np.float32)
    yc = np.empty((2, 2, CTX, 512), np.float32)
    for k in range(NCORE):
        b, d, g0 = k // 4, (k % 4) // 2, (k % 2) * 16
        Yk = res[k]["Y"]
        for u in range(NU):
            g = g0 + u
            seq = Yk[u].reshape(8, 16, NB1).transpose(2, 0, 1).reshape(NB1 * 8, 16)
            c_part, l_part = seq[:CTX], seq[CTX:]
            if d == 1:
                c_part, l_part = c_part[::-1], l_part[::-1]
            y[d, b, :, g * 16:(g + 1) * 16] = l_part
            yc[d, b, :, g * 16:(g + 1) * 16] = c_part
    return y, yc


NREST = IN_COLS - 512


def build_kc1(ntok, rowlen):
    k = MK()
    nc = k.nc
    TT = 256
    nch = ntok // TT
    R = TT // rowlen
    L = rowlen
    xT = k.dram("xT", [128, KC, ntok], F32, "ExternalInput")
    prm = k.dram("prm", [128, 4, KC], F32, "ExternalInput")
    cprm = k.dram("cprm", [128, 4, 4], F32, "ExternalInput")
    cw = k.dram("cw", [128, 4, 31], F32, "ExternalInput")
    w_in = k.dram("w_in", [128, KC, NREST], F32, "ExternalInput")
    w_glu = k.dram("w_glu", [128, 4, 1024], F32, "ExternalInput")
    w_a = k.dram("w_a", [128, 4, 1024], F32, "ExternalInput")
    w_b = k.dram("w_b", [128, 4, 1024], F32, "ExternalInput")
    w_o = k.dram("w_o", [128, KC, 1024], F32, "ExternalInput")
    uT = k.dram("uT", [128, 4, ntok], F32, "ExternalInput")
    yfT = k.dram("yfT", [128, 4, ntok], F32, "ExternalInput")
    ybT = k.dram("ybT", [128, 4, ntok], F32, "ExternalInput")
    xo = k.dram("xo", [128, KC, ntok], F32, "ExternalOutput")

    wint = k.sb([128, KC, NREST], BF16)
    wglut = k.sb([128, 4, 1024], BF16)
    wat = k.sb([128, 4, 1024], BF16)
    wbt = k.sb([128, 4, 1024], BF16)
    wot = k.sb([128, KC, 1024], BF16)
    pt = k.sb([128, 4, KC], F32)
    cpt = k.sb([128, 4, 4], F32)
    cwt = k.sb([128, 4, 31], F32)
    gs = k.sb([128, KC], F32)
    ones_bf = k.sb([128, 128], BF16)
    ones_ln = k.sb([128, 128], F32)
    eps_t = k.sb([128, 1], F32)
    k.dma('sp', pt[:], prm[:, :, :], writes=['prm'])
    k.dma('sp', cpt[:], cprm[:, :, :], writes=['cprm'])
    k.dma('sp', cwt[:], cw[:, :, :], writes=['cw'])
    for c in range(KC):
        k.dma('pool', wint[:, c, :], w_in[:, c, :], writes=['win%d' % c])
    k.dma('pool', wglut[:], w_glu[:, :, :], writes=['wglu'])
    k.dma('pool', wat[:], w_a[:, :, :], writes=['wa'])
    k.dma('pool', wbt[:], w_b[:, :, :], writes=['wb'])
    k.dma('pool', wot[:], w_o[:, :, :], writes=['wo'])
    k.op('dve', lambda: nc.vector.memset(ones_bf[:], 1.0), writes=['ones_bf'])
    k.op('dve', lambda: nc.vector.memset(ones_ln[:], 1.0 / 512.0), writes=['ones_ln'])
    k.op('dve', lambda: nc.vector.memset(eps_t[:], EPS), writes=['eps'])
    emit_gs(k, pt[:, 0, :], pt[:, 1, :], gs, '')

    xts = [k.sb([128, KC, TT], F32, name="x%d" % i) for i in range(2)]
    sq = k.sb([128, KC, TT], BF16)
    rstd = k.sb([128, TT], F32)
    tmp = k.sb([128, KC, TT], F32)
    h = k.sb([128, KC, TT], BF16)
    vpad = k.sb([128, 4, R, L + 30], F32)
    co = k.sb([128, 4, R, L], F32)
    csq = k.sb([128, 4, TT], F32)
    lnr = k.sb([128, TT], F32)
    lnm2 = k.sb([128, TT], F32)
    cb = k.sb([128, 4, TT], BF16)
    sg = [k.sb([128, TT], F32, name="sg%d" % i) for i in range(2)]
    ut = k.sb([128, 4, TT], F32)
    yft = k.sb([128, 4, TT], F32)
    ybt = k.sb([128, 4, TT], F32)
    gin = k.sb([128, 4, TT], BF16)
    v2 = k.sb([128, 4, TT], BF16)
    mb = k.sb([128, KC, TT], F32)
    mg = k.sb([128, KC, TT], BF16)
    ps_ss = k.ps([128, 512], F32)
    pA = [k.ps([128, 512], F32, name="pA%d" % i) for i in range(3)]
    pB = [k.ps([128, 512], F32, name="pB%d" % i) for i in range(3)]
    k.op('pool', lambda: nc.gpsimd.memset(vpad[:], 0.0), writes=['vpad'])
    cnt = [0]

    def mm_group(ps_t, pkey, wtile, wkey, col0, rhs_t, rkey, nk):
        for c in range(nk):
            k.op('pe', lambda c=c: nc.tensor.matmul(ps_t[:, :TT], lhsT=wtile[:, c, col0:col0 + 128], rhs=rhs_t[:, c, :],
                                                    start=(c == 0), stop=(c == nk - 1)),
                 reads=[rkey] + ([wkey] if isinstance(wkey, str) else ['win%d' % c]), writes=[pkey])

    def nxt():
        i = cnt[0] % 3
        cnt[0] += 1
        return i

    for ci in range(nch):
        xt = xts[ci % 2]
        xkey = 'x%d' % (ci % 2)
        tsl = slice(ci * TT, (ci + 1) * TT)
        k.dma('sp', xt[:], xT[:, :, tsl], writes=[xkey])
        k.dma('sp', ut[:], uT[:, :, tsl], writes=['ut'])
        k.dma('sp', yft[:], yfT[:, :, tsl], writes=['yft'])
        k.dma('sp', ybt[:], ybT[:, :, tsl], writes=['ybt'])
        emit_norm_mod(k, xt, xkey, h, 'h', TT, gs, pt[:, 2, :], ones_bf, sq, rstd, tmp, ps_ss, eps_t, '')
        for m in range(4):
            i = nxt()
            mm_group(pA[i], 'pA%d' % i, wint, None, m * 128, h, 'h', KC)
            mm_group(pB[i], 'pB%d' % i, wint, None, (4 + m) * 128, h, 'h', KC)
            s_ = sg[m % 2]
            skey = 'sg%d' % (m % 2)
            k.op('act', lambda i=i, s_=s_: nc.scalar.activation(out=s_[:], in_=pB[i][:, :TT], func=AF.Sigmoid),
                 reads=['pB%d' % i], writes=[skey])
            k.op('dve', lambda i=i, s_=s_, m=m: nc.vector.tensor_tensor(
                out=vpad[:, m, :, 15:15 + L], in0=pA[i][:, :TT].rearrange("p (r l) -> p r l", l=L),
                in1=s_[:].rearrange("p (r l) -> p r l", l=L), op=ALU.mult),
                reads=['pA%d' % i, skey], writes=['vpad'])
        for m in range(4):
            k.op('dve', lambda m=m: nc.vector.tensor_scalar(out=co[:, m], in0=vpad[:, m, :, 0:L], scalar1=cwt[:, m, 0:1],
                                                            scalar2=cpt[:, 1, m:m + 1], op0=ALU.mult, op1=ALU.add),
                 reads=['vpad', 'cw', 'cprm'], writes=['co%d' % m])
            for t in range(1, 31):
                k.op('dve', lambda m=m, t=t: nc.vector.scalar_tensor_tensor(out=co[:, m], in0=vpad[:, m, :, t:t + L],
                                                                           scalar=cwt[:, m, t:t + 1], in1=co[:, m],
                                                                           op0=ALU.mult, op1=ALU.add),
                     reads=['vpad', 'cw', 'co%d' % m], writes=['co%d' % m])
        for m in range(4):
            k.op('act', lambda m=m: nc.scalar.activation(out=csq[:, m, :], in_=co[:, m].rearrange("p r l -> p (r l)"),
                                                         func=AF.Square), reads=['co%d' % m], writes=['csq'])
        i = nxt()
        for m in range(4):
            k.op('pe', lambda m=m, i=i: nc.tensor.matmul(pA[i][:, :TT], lhsT=ones_ln[:],
                                                         rhs=co[:, m].rearrange("p r l -> p (r l)"),
                                                         start=(m == 0), stop=(m == 3)),
                 reads=['co%d' % m, 'ones_ln'], writes=['pA%d' % i])
        for m in range(4):
            k.op('pe', lambda m=m, i=i: nc.tensor.matmul(pB[i][:, :TT], lhsT=ones_ln[:], rhs=csq[:, m, :],
                                                         start=(m == 0), stop=(m == 3)),
                 reads=['csq', 'ones_ln'], writes=['pB%d' % i])
        k.op('act', lambda i=i: nc.scalar.activation(out=lnm2[:], in_=pA[i][:, :TT], func=AF.Square),
             reads=['pA%d' % i], writes=['lnm2'])
        k.op('dve', lambda i=i: nc.vector.tensor_tensor(out=lnr[:], in0=pB[i][:, :TT], in1=lnm2[:], op=ALU.subtract),
             reads=['pB%d' % i, 'lnm2'], writes=['lnr'])
        k.op('act', lambda: nc.scalar.activation(out=lnr[:], in_=lnr[:], func=AF.Sqrt, bias=eps_t[:, 0:1], scale=1.0),
             reads=['lnr', 'eps'], writes=['lnr'])
        k.op('dve', lambda: nc.vector.reciprocal(out=lnr[:], in_=lnr[:]), reads=['lnr'], writes=['lnr'])
        for m in range(4):
            cm = co[:, m].rearrange("p r l -> p (r l)")
            k.op('dve', lambda cm=cm, i=i: nc.vector.tensor_tensor(out=cm, in0=cm, in1=pA[i][:, :TT], op=ALU.subtract),
                 reads=['co%d' % m, 'pA%d' % i], writes=['co%d' % m])
            k.op('dve', lambda cm=cm: nc.vector.tensor_tensor(out=cm, in0=cm, in1=lnr[:], op=ALU.mult),
                 reads=['co%d' % m, 'lnr'], writes=['co%d' % m])
            k.op('act', lambda cm=cm, m=m: nc.scalar.activation(out=cb[:, m, :], in_=cm, func=AF.Silu,
                                                                bias=cpt[:, 3, m:m + 1], scale=cpt[:, 2, m:m + 1]),
                 reads=['co%d' % m, 'cprm'], writes=['cb'])
        for m in range(KC):
            i = nxt()
            mm_group(pA[i], 'pA%d' % i, wbt, 'wb', m * 128, cb, 'cb', 4)
            mm_group(pB[i], 'pB%d' % i, wint, None, 1024 + 1024 + m * 128, h, 'h', KC)
            s_ = sg[m % 2]
            skey = 'sg%d' % (m % 2)
            k.op('act', lambda i=i, s_=s_: nc.scalar.activation(out=s_[:], in_=pB[i][:, :TT], func=AF.Sigmoid),
                 reads=['pB%d' % i], writes=[skey])
            k.op('dve', lambda i=i, s_=s_, m=m: nc.vector.tensor_tensor(out=mb[:, m, :], in0=pA[i][:, :TT], in1=s_[:],
                                                                        op=ALU.mult),
                 reads=['pA%d' % i, skey], writes=['mb'])
        for m in range(4):
            k.op('dve', lambda m=m: nc.vector.scalar_tensor_tensor(out=ut[:, m, :], in0=ut[:, m, :], scalar=cpt[:, 0, m:m + 1],
                                                                   in1=yft[:, m, :], op0=ALU.mult, op1=ALU.add),
                 reads=['ut', 'yft', 'cprm'], writes=['ut'])
            k.op('dve', lambda m=m: nc.vector.tensor_tensor(out=ut[:, m, :], in0=ut[:, m, :], in1=ybt[:, m, :], op=ALU.add),
                 reads=['ut', 'ybt'], writes=['ut'])
            k.op('act', lambda m=m: nc.scalar.activation(out=gin[:, m, :], in_=ut[:, m, :], func=AF.Gelu_apprx_tanh),
                 reads=['ut'], writes=['gin'])
        for m in range(4):
            i = nxt()
            mm_group(pA[i], 'pA%d' % i, wglut, 'wglu', m * 128, gin, 'gin', 4)
            mm_group(pB[i], 'pB%d' % i, wglut, 'wglu', (4 + m) * 128, gin, 'gin', 4)
            s_ = sg[m % 2]
            skey = 'sg%d' % (m % 2)
            k.op('act', lambda i=i, s_=s_: nc.scalar.activation(out=s_[:], in_=pB[i][:, :TT], func=AF.Sigmoid),
                 reads=['pB%d' % i], writes=[skey])
            k.op('dve', lambda i=i, s_=s_, m=m: nc.vector.tensor_tensor(out=v2[:, m, :], in0=pA[i][:, :TT], in1=s_[:],
                                                                        op=ALU.mult),
                 reads=['pA%d' % i, skey], writes=['v2'])
        for m in range(KC):
            i = nxt()
            mm_group(pA[i], 'pA%d' % i, wat, 'wa', m * 128, v2, 'v2', 4)
            mm_group(pB[i], 'pB%d' % i, wint, None, 1024 + m * 128, h, 'h', KC)
            s_ = sg[m % 2]
            skey = 'sg%d' % (m % 2)
            k.op('act', lambda i=i, s_=s_: nc.scalar.activation(out=s_[:], in_=pB[i][:, :TT], func=AF.Sigmoid),
                 reads=['pB%d' % i], writes=[skey])
            k.op('dve', lambda i=i, s_=s_, m=m: nc.vector.tensor_tensor(out=s_[:], in0=pA[i][:, :TT], in1=s_[:],
                                                                        op=ALU.mult),
                 reads=['pA%d' % i, skey], writes=[skey])
            k.op('dve', lambda s_=s_, m=m: nc.vector.tensor_tensor(out=mg[:, m, :], in0=s_[:], in1=mb[:, m, :], op=ALU.add),
                 reads=[skey, 'mb'], writes=['mg'])
        for m in range(KC):
            i = nxt()
            mm_group(pA[i], 'pA%d' % i, wot, 'wo', m * 128, mg, 'mg', KC)
            k.op('dve', lambda i=i, m=m, xt=xt: nc.vector.scalar_tensor_tensor(out=xt[:, m, :], in0=pA[i][:, :TT],
                                                                               scalar=pt[:, 3, m:m + 1], in1=xt[:, m, :],
                                                                               op0=ALU.mult, op1=ALU.add),
                 reads=['pA%d' % i, xkey, 'prm'], writes=[xkey])
        k.dma('sp', xo[:, :, tsl], xt[:], reads=[xkey], is_output=True)
    return k.finish()


def kc1_weights(inp, l):
    w_in_rest = rows_pm(inp['w_in'][l][:, 512:])
    return {"w_in": w_in_rest, "w_glu": rows_pm(inp['w_glu'][l]), "w_a": rows_pm(inp['w_a_out'][l]),
            "w_b": rows_pm(inp['w_b_out'][l]), "w_o": rows_pm(inp['w_o'][l]),
            "cw": np.ascontiguousarray(inp['conv_w'][l].T.reshape(4, 128, 31).transpose(1, 0, 2)),
            "cprm": np.ascontiguousarray(np.stack([col_pm(inp['s5_d'][l]), col_pm(inp['conv_b'][l]),
                                                   col_pm(inp['conv_ln_g'][l]), col_pm(inp['conv_ln_b'][l])], axis=1))}


def run_kc1(x_list, u_list, yf_list, yb_list, prm_list, wts, ntok, rowlen):
    key = ('kc1', ntok, rowlen)
    if key not in _CACHE:
        _CACHE[key] = build_kc1(ntok, rowlen)
    in_maps = []
    for k in range(NCORE):
        m = dict(wts)
        m.update({"xT": to_fm(x_list[k]), "uT": to_fm(u_list[k]), "yfT": to_fm(yf_list[k]), "ybT": to_fm(yb_list[k]),
                  "prm": prm_list[k]})
        in_maps.append(m)
    res = run(_CACHE[key], in_maps)
    return [from_fm(r["xo"]) for r in res]


NEXP = 32
BIG = 1.0e30


def build_kc2(ntok, final):
    k = MK()
    nc = k.nc
    npass = max(1, ntok // 2048)
    NTP = ntok // npass
    CH = min(512, NTP)
    nchp = NTP // CH
    NSUB = CH // 128
    NTL = NTP // 128
    xT = k.dram("xT", [128, KC, ntok], F32, "ExternalInput")
    xtm = k.dram("xtm", [128, ntok // 128, D], F32, "ExternalInput")
    prm = k.dram("prm", [128, 3, KC], F32, "ExternalInput")
    rowp = k.dram("rowp", [128, 2, D], F32, "ExternalInput")
    wr = k.dram("wr", [128, KC, 36], F32, "ExternalInput")
    br = k.dram("br", [1, 36], F32, "ExternalInput")
    wg = k.dram("wg", [NEXP, 128, KC, 256], F32, "ExternalInput")
    wu = k.dram("wu", [NEXP, 128, KC, 256], F32, "ExternalInput")
    wd = k.dram("wd", [NEXP, 128, 2, D], F32, "ExternalInput")
    xo = k.dram("xo", [128, ntok // 128, D], F32, "ExternalOutput")

    pt = k.sb([128, 3, KC], F32)
    rowt = k.sb([128, 2, D], F32)
    wrt = k.sb([128, KC, 36], F32)
    brt = k.sb([1, 36], F32)
    gs = k.sb([128, KC], F32)
    ones_bf = k.sb([128, 128], BF16)
    ones_r = k.sb([1, 128], F32)
    eps_t = k.sb([128, 1], F32)
    k.dma('sp', pt[:], prm[:, :, :], writes=['prm'])
    k.dma('sp', rowt[:], rowp[:, :, :], writes=['rowp'])
    k.dma('sp', wrt[:], wr[:, :, :], writes=['wr'])
    k.dma('sp', brt[:], br[:, :], writes=['br'])
    k.op('dve', lambda: nc.vector.memset(ones_bf[:], 1.0), writes=['ones_bf'])
    k.op('dve', lambda: nc.vector.memset(ones_r[:], 1.0), writes=['ones_r'])
    k.op('dve', lambda: nc.vector.memset(eps_t[:], EPS), writes=['eps'])
    emit_gs(k, pt[:, 0, :], pt[:, 1, :], gs, '')

    xc = k.sb([128, KC, CH], F32)
    sq = k.sb([128, KC, CH], BF16)
    rstd = k.sb([128, CH], F32)
    tmp = k.sb([128, KC, CH], F32)
    h2f = k.sb([128, KC, CH], F32)
    h2T = k.sb([128, KC, NTP], BF16)
    gw = k.sb([128, NTL, NEXP], F32)
    acc = k.sb([128, NTL, D], F32)
    wgt = [k.sb([128, KC, 256], BF16, name="wg%d" % i) for i in range(2)]
    wut = [k.sb([128, KC, 256], BF16, name="wu%d" % i) for i in range(2)]
    wdt = [k.sb([128, 2, D], BF16, name="wd%d" % i) for i in range(2)]
    sil = [k.sb([128, CH], F32, name="sil%d" % i) for i in range(2)]
    actT = [k.sb([128, 2, CH], BF16, name="actT%d" % i) for i in range(2)]
    xin = [k.sb([128, D], F32, name="xin%d" % i) for i in range(2)]
    lg = k.sb([128, 36], F32)
    r1 = k.sb([128, 8], F32)
    gex = k.sb([128, 4], F32)
    gmask = k.sb([128, 4], F32)
    leff = k.sb([128, 4, 8], F32)
    top8 = k.sb([128, 8], F32)
    g1 = k.sb([128, NEXP], F32)
    g2 = k.sb([128, NEXP], F32)
    ps_ss = k.ps([128, 512], F32)
    ps_r = k.ps([128, 512], F32)
    pg = [k.ps([128, 512], F32, name="pg%d" % i) for i in range(2)]
    pu = [k.ps([128, 512], F32, name="pu%d" % i) for i in range(2)]
    po = [k.ps([128, 512], F32, name="po%d" % i) for i in range(2)]

    def V(fn, reads, writes, e='dve'):
        return k.op(e, fn, reads=reads, writes=writes)

    for ps_i in range(npass):
        t0 = ps_i * NTP
        for ci in range(nchp):
            c0 = t0 + ci * CH
            k.dma('sp', xc[:], xT[:, :, c0:c0 + CH], writes=['xc'])
            emit_norm_mod(k, xc, 'xc', h2f, 'h2f', CH, gs, pt[:, 2, :], ones_bf, sq, rstd, tmp, ps_ss, eps_t, '')
            V(lambda ci=ci: nc.gpsimd.tensor_copy(out=h2T[:, :, ci * CH:(ci + 1) * CH], in_=h2f[:]), ['h2f'], ['h2T'], 'pool')
            for sub in range(NSUB):
                tl = ci * NSUB + sub
                for c in range(KC):
                    k.op('pe', lambda c=c, sub=sub: nc.tensor.matmul(ps_r[:, 0:36], lhsT=h2f[:, c, sub * 128:(sub + 1) * 128],
                                                                     rhs=wrt[:, c, :], start=(c == 0), stop=False),
                         reads=['h2f', 'wr'], writes=['ps_r'])
                k.op('pe', lambda: nc.tensor.matmul(ps_r[:, 0:36], lhsT=ones_r[:], rhs=brt[:], start=False, stop=True),
                     reads=['ones_r', 'br'], writes=['ps_r'])
                V(lambda: nc.vector.tensor_copy(out=lg[:], in_=ps_r[:, 0:36]), ['ps_r'], ['lg'])
                V(lambda: nc.vector.tensor_reduce(out=r1[:, 0:1], in_=lg[:, 0:4], axis=mybir.AxisListType.X, op=ALU.max),
                  ['lg'], ['r1'])
                V(lambda: nc.vector.tensor_scalar(out=gmask[:], in0=lg[:, 0:4], scalar1=r1[:, 0:1], scalar2=None,
                                                  op0=ALU.is_equal), ['lg', 'r1'], ['gmask'])
                V(lambda: nc.vector.tensor_scalar(out=r1[:, 1:2], in0=r1[:, 0:1], scalar1=-1.0, scalar2=None, op0=ALU.mult),
                  ['r1'], ['r1'])
                V(lambda: nc.scalar.activation(out=gex[:], in_=lg[:, 0:4], func=AF.Exp, bias=r1[:, 1:2], scale=1.0,
                                               accum_out=r1[:, 2:3]), ['lg', 'r1'], ['gex', 'r1'], 'act')
                V(lambda: nc.vector.reciprocal(out=r1[:, 3:4], in_=r1[:, 2:3]), ['r1'], ['r1'])
                V(lambda: nc.vector.tensor_scalar(out=gmask[:], in0=gmask[:], scalar1=BIG, scalar2=-BIG, op0=ALU.mult,
                                                  op1=ALU.add), ['gmask'], ['gmask'])
                V(lambda: nc.vector.tensor_tensor(out=leff[:], in0=lg[:, 4:36].rearrange("p (g e) -> p g e", e=8),
                                                  in1=gmask[:].unsqueeze(2).to_broadcast([128, 4, 8]), op=ALU.add),
                  ['lg', 'gmask'], ['leff'])
                V(lambda: nc.vector.max(out=top8[:], in_=leff[:].rearrange("p g e -> p (g e)")), ['leff'], ['top8'])
                V(lambda: nc.vector.tensor_tensor(out=r1[:, 4:5], in0=top8[:, 1:2], in1=top8[:, 0:1], op=ALU.subtract),
                  ['top8', 'r1'], ['r1'])
                V(lambda: nc.scalar.activation(out=r1[:, 5:6], in_=r1[:, 4:5], func=AF.Exp), ['r1'], ['r1'], 'act')
                V(lambda: nc.vector.tensor_scalar(out=r1[:, 6:7], in0=r1[:, 5:6], scalar1=1.0, scalar2=None, op0=ALU.add),
                  ['r1'], ['r1'])
                V(lambda: nc.vector.reciprocal(out=r1[:, 6:7], in_=r1[:, 6:7]), ['r1'], ['r1'])
                V(lambda: nc.vector.tensor_tensor(out=r1[:, 7:8], in0=r1[:, 6:7], in1=r1[:, 5:6], op=ALU.mult),
                  ['r1'], ['r1'])
                V(lambda: nc.vector.tensor_tensor(out=r1[:, 6:8], in0=r1[:, 6:8],
                                                  in1=r1[:, 3:4].to_broadcast([128, 2]), op=ALU.mult), ['r1'], ['r1'])
                V(lambda: nc.vector.tensor_scalar(out=g1[:], in0=leff[:].rearrange("p g e -> p (g e)"), scalar1=top8[:, 0:1],
                                                  scalar2=r1[:, 6:7], op0=ALU.is_equal, op1=ALU.mult),
                  ['leff', 'top8', 'r1'], ['g1'])
                V(lambda: nc.vector.tensor_scalar(out=g2[:], in0=leff[:].rearrange("p g e -> p (g e)"), scalar1=top8[:, 1:2],
                                                  scalar2=r1[:, 7:8], op0=ALU.is_equal, op1=ALU.mult),
                  ['leff', 'top8', 'r1'], ['g2'])
                V(lambda tl=tl: nc.vector.tensor_tensor(out=gw[:, tl, :], in0=g1[:], in1=g2[:], op=ALU.add),
                  ['g1', 'g2'], ['gw'])
        for e in range(NEXP):
            b_ = e % 2
            k.dma('pool', wgt[b_][:], wg[e], writes=['wg%d' % b_])
            k.dma('pool', wut[b_][:], wu[e], writes=['wu%d' % b_])
            k.dma('pool', wdt[b_][:], wd[e], writes=['wd%d' % b_])
            for ci in range(nchp):
                a_ = (e * nchp + ci) % 2
                for hf in range(2):
                    for c in range(KC):
                        k.op('pe', lambda c=c, hf=hf: nc.tensor.matmul(pg[hf][:, :CH], lhsT=wgt[b_][:, c, hf * 128:(hf + 1) * 128],
                                                                       rhs=h2T[:, c, ci * CH:(ci + 1) * CH],
                                                                       start=(c == 0), stop=(c == KC - 1)),
                             reads=['wg%d' % b_, 'h2T'], writes=['pg%d' % hf])
                    for c in range(KC):
                        k.op('pe', lambda c=c, hf=hf: nc.tensor.matmul(pu[hf][:, :CH], lhsT=wut[b_][:, c, hf * 128:(hf + 1) * 128],
                                                                       rhs=h2T[:, c, ci * CH:(ci + 1) * CH],
                                                                       start=(c == 0), stop=(c == KC - 1)),
                             reads=['wu%d' % b_, 'h2T'], writes=['pu%d' % hf])
                    k.op('act', lambda hf=hf: nc.scalar.activation(out=sil[hf][:], in_=pg[hf][:, :CH], func=AF.Silu),
                         reads=['pg%d' % hf], writes=['sil%d' % hf])
                    k.op('dve', lambda hf=hf: nc.vector.tensor_tensor(out=actT[a_][:, hf, :], in0=pu[hf][:, :CH], in1=sil[hf][:],
                                                                      op=ALU.mult),
                         reads=['pu%d' % hf, 'sil%d' % hf], writes=['actT%d' % a_])
                for sub in range(NSUB):
                    tl = ci * NSUB + sub
                    for nh in range(2):
                        p_ = po[nh]
                        for kh in range(2):
                            k.op('pe', lambda kh=kh, nh=nh, sub=sub, p_=p_: nc.tensor.matmul(
                                p_[:, :], lhsT=actT[a_][:, kh, sub * 128:(sub + 1) * 128],
                                rhs=wdt[b_][:, kh, nh * 512:(nh + 1) * 512], start=(kh == 0), stop=(kh == 1)),
                                reads=['actT%d' % a_, 'wd%d' % b_], writes=['po%d' % nh])
                        if e == 0:
                            k.op('dve', lambda nh=nh, tl=tl, p_=p_: nc.vector.tensor_scalar(
                                out=acc[:, tl, nh * 512:(nh + 1) * 512], in0=p_[:, :], scalar1=gw[:, tl, e:e + 1],
                                scalar2=None, op0=ALU.mult),
                                reads=['po%d' % nh, 'gw'], writes=['acc%d' % tl])
                        else:
                            k.op('dve', lambda nh=nh, tl=tl, p_=p_, e=e: nc.vector.scalar_tensor_tensor(
                                out=acc[:, tl, nh * 512:(nh + 1) * 512], in0=p_[:, :], scalar=gw[:, tl, e:e + 1],
                                in1=acc[:, tl, nh * 512:(nh + 1) * 512], op0=ALU.mult, op1=ALU.add),
                                reads=['po%d' % nh, 'gw', 'acc%d' % tl], writes=['acc%d' % tl])
        for tl in range(NTL):
            gt = ps_i * NTL + tl
            xi = xin[tl % 2]
            xk = 'xin%d' % (tl % 2)
            k.dma('sp', xi[:], xtm[:, gt, :], writes=[xk])
            k.op('pool', lambda tl=tl: nc.gpsimd.tensor_tensor(out=acc[:, tl, :], in0=acc[:, tl, :], in1=rowt[:, 0, :],
                                                               op=ALU.mult),
                 reads=['acc%d' % tl, 'rowp'], writes=['acc%d' % tl])
            k.op('pool', lambda tl=tl, xi=xi: nc.gpsimd.tensor_tensor(out=acc[:, tl, :], in0=acc[:, tl, :], in1=xi[:],
                                                                      op=ALU.add),
                 reads=['acc%d' % tl, xk], writes=['acc%d' % tl])
            if final:
                k.op('act', lambda tl=tl, xi=xi: nc.scalar.activation(out=xi[:], in_=acc[:, tl, :], func=AF.Square,
                                                                      accum_out=r1[:, 0:1]),
                     reads=['acc%d' % tl, xk, 'r1'], writes=[xk, 'r1'])
                k.op('act', lambda: nc.scalar.activation(out=r1[:, 1:2], in_=r1[:, 0:1], func=AF.Sqrt, bias=eps_t[:, 0:1],
                                                         scale=1.0 / D), reads=['r1', 'eps'], writes=['r1'])
                k.op('dve', lambda: nc.vector.reciprocal(out=r1[:, 1:2], in_=r1[:, 1:2]), reads=['r1'], writes=['r1'])
                k.op('dve', lambda tl=tl: nc.vector.scalar_tensor_tensor(out=acc[:, tl, :], in0=acc[:, tl, :],
                                                                         scalar=r1[:, 1:2], in1=rowt[:, 1, :],
                                                                         op0=ALU.mult, op1=ALU.mult),
                     reads=['acc%d' % tl, 'r1', 'rowp'], writes=['acc%d' % tl])
            k.dma('sp', xo[:, gt, :], acc[:, tl, :], reads=['acc%d' % tl], is_output=True)
    return k.finish()


def kc2_weights(inp, l):
    wr = np.concatenate([inp['w_route_group'][l], inp['w_route_expert'][l]], axis=1)
    br = np.concatenate([inp['b_route_group'][l], inp['b_route_expert'][l]])[None, :]
    wg = np.stack([rows_pm(inp['w_exp_gate'][l, e]) for e in range(NEXP)])
    wu = np.stack([rows_pm(inp['w_exp_up'][l, e]) for e in range(NEXP)])
    wd = np.stack([rows_pm(inp['w_exp_down'][l, e]) for e in range(NEXP)])
    return {"wr": rows_pm(wr), "br": np.ascontiguousarray(br), "wg": wg, "wu": wu, "wd": wd}


def tok_tiles(a):
    n, f = a.shape
    return np.ascontiguousarray(a.reshape(n // 128, 128, f).transpose(1, 0, 2))


def from_tok_tiles(a):
    p, t, f = a.shape
    return np.ascontiguousarray(a.transpose(1, 0, 2).reshape(t * 128, f))


def run_kc2(x_list, prm_list, rowp_list, wts, ntok, final):
    key = ('kc2', ntok, final)
    if key not in _CACHE:
        _CACHE[key] = build_kc2(ntok, final)
    in_maps = []
    for k in range(NCORE):
        m = dict(wts)
        m.update({"xT": to_fm(x_list[k]), "xtm": tok_tiles(x_list[k]), "prm": prm_list[k], "rowp": rowp_list[k]})
        in_maps.append(m)
    res = run(_CACHE[key], in_maps)
    return [from_tok_tiles(r["xo"]) for r in res]


def _prm(vecs):
    return np.ascontiguousarray(np.stack([col_pm(v) for v in vecs], axis=1))


def kernel(x, c, ctx, c_ctx, w_ada, b_ada, g_mix, g_ffn, w_in, s5_lam_re, s5_lam_im, s5_log_step,
           s5_b_re, s5_b_im, s5_c_re, s5_c_im, s5_d, w_glu, w_a_out, conv_w, conv_b, conv_ln_g, conv_ln_b,
           w_b_out, w_o, w_route_group, b_route_group, w_route_expert, b_route_expert,
           w_exp_gate, w_exp_up, w_exp_down, g_final):
    inp = dict(x=x, c=c, ctx=ctx, c_ctx=c_ctx, w_ada=w_ada, b_ada=b_ada, g_mix=g_mix, g_ffn=g_ffn, w_in=w_in,
               s5_lam_re=s5_lam_re, s5_lam_im=s5_lam_im, s5_log_step=s5_log_step, s5_b_re=s5_b_re, s5_b_im=s5_b_im,
               s5_c_re=s5_c_re, s5_c_im=s5_c_im, s5_d=s5_d, w_glu=w_glu, w_a_out=w_a_out, conv_w=conv_w,
               conv_b=conv_b, conv_ln_g=conv_ln_g, conv_ln_b=conv_ln_b, w_b_out=w_b_out, w_o=w_o,
               w_route_group=w_route_group, b_route_group=b_route_group, w_route_expert=w_route_expert,
               b_route_expert=b_route_expert, w_exp_gate=w_exp_gate, w_exp_up=w_exp_up, w_exp_down=w_exp_down,
               g_final=g_final)
    inp = {k_: np.asarray(v, dtype=np.float32) for k_, v in inp.items()}
    ada = run_ada(inp['c'], inp['c_ctx'], inp['w_ada'], inp['b_ada'])
    TPC = SEQ // 4
    xs = [np.ascontiguousarray(inp['x'][k // 4, (k % 4) * TPC:(k % 4 + 1) * TPC]) for k in range(NCORE)]
    xcs = [np.ascontiguousarray(inp['ctx'][b]) for b in range(2)]

    def mv(l, row, i):
        return ada[l, row, i * D:(i + 1) * D]

    for l in range(2):
        last = (l == 1)
        prm_lat = [_prm([inp['g_mix'][l], mv(l, k // 4, 1), mv(l, k // 4, 0)]) for k in range(NCORE)]
        prm_ctx = [_prm([inp['g_mix'][l], mv(l, 2, 1), mv(l, 2, 0)]) for k in range(NCORE)]
        w_u = np.ascontiguousarray(inp['w_in'][l][:, :512])
        uT_lat = run_ka(xs, prm_lat, w_u, TPC)
        uT_ctx = run_ka([xcs[k // 4] for k in range(NCORE)], prm_ctx, w_u, CTX)
        u_lat = [from_fm(t) for t in uT_lat]
        u_ctx = [from_fm(uT_ctx[0]), from_fm(uT_ctx[4])]
        z_lat = np.stack([np.concatenate(u_lat[b * 4:(b + 1) * 4], axis=0) for b in range(2)])
        z_ctx = np.stack(u_ctx)
        y, yc = run_kb(l, z_lat, z_ctx, inp)
        wts1 = kc1_weights(inp, l)
        prm1_lat = [_prm([inp['g_mix'][l], mv(l, k // 4, 1), mv(l, k // 4, 0), mv(l, k // 4, 2)]) for k in range(NCORE)]
        yf = [y[0, k // 4, (k % 4) * TPC:(k % 4 + 1) * TPC] for k in range(NCORE)]
        yb = [y[1, k // 4, (k % 4) * TPC:(k % 4 + 1) * TPC] for k in range(NCORE)]
        xs_new = run_kc1(xs, u_lat, yf, yb, prm1_lat, wts1, TPC, 64)
        if not last:
            prm1_ctx = [_prm([inp['g_mix'][l], mv(l, 2, 1), mv(l, 2, 0), mv(l, 2, 2)]) for k in range(NCORE)]
            r = run_kc1([xcs[k // 4] for k in range(NCORE)], [u_ctx[k // 4] for k in range(NCORE)],
                        [yc[0, k // 4] for k in range(NCORE)], [yc[1, k // 4] for k in range(NCORE)],
                        prm1_ctx, wts1, CTX, CTX)
            xcs = [r[0], r[4]]
        xs = xs_new
        wts2 = kc2_weights(inp, l)
        prm2_lat = [_prm([inp['g_ffn'][l], mv(l, k // 4, 4), mv(l, k // 4, 3)]) for k in range(NCORE)]
        rowp_lat = [np.ascontiguousarray(np.broadcast_to(np.stack([mv(l, k // 4, 5), inp['g_final']])[None], (128, 2, D)))
                    for k in range(NCORE)]
        xs = run_kc2(xs, prm2_lat, rowp_lat, wts2, TPC, last)
        if not last:
            prm2_ctx = [_prm([inp['g_ffn'][l], mv(l, 2, 4), mv(l, 2, 3)]) for k in range(NCORE)]
            rowp_ctx = [np.ascontiguousarray(np.broadcast_to(np.stack([mv(l, 2, 5), inp['g_final']])[None], (128, 2, D)))
                        for k in range(NCORE)]
            r = run_kc2([xcs[k // 4] for k in range(NCORE)], prm2_ctx, rowp_ctx, wts2, CTX, False)
            xcs = [r[0], r[4]]
    out = np.stack([np.concatenate(xs[b * 4:(b + 1) * 4], axis=0) for b in range(2)])
    return out.astype(np.float32)
```
